# Optimizing a Trainium2 kernel written in Bass

```python
import jax, jax.numpy as jnp
from jax import lax
import numpy as np

D_MODEL = 1024
BATCH = 4
SEQ = 8192
DEPTH = 1

N_META = 16
EPS = 1e-6
MIX_WIDTH = D_MODEL
CONV_CH = MIX_WIDTH // 2
CONV_HEADS = 8
CONV_WIDTH = 3
POOL_CH = MIX_WIDTH - CONV_CH
POOL_WINDOWS = (2, 4, 8, 16)
N_POOL_GROUPS = len(POOL_WINDOWS)
POOL_GROUP_DIM = POOL_CH // N_POOL_GROUPS
IN_PROJ_COLS = 3 * CONV_CH + POOL_CH

PEER_HEADS = 8
N_KEYS = 128
N_EXPERTS = N_KEYS * N_KEYS
PEER_KEY_DIM = 256
PEER_KEY_HALF = PEER_KEY_DIM // 2
PEER_TOPK = 16
PEER_CHUNK = 128

kernel_name = "hybrid_conv_pool_peer_encoder"


def rms_norm(x, g):
    xf = x.astype(jnp.float32)
    y = xf * lax.rsqrt(jnp.mean(xf * xf, axis=-1, keepdims=True) + EPS)
    return (y * g.astype(jnp.float32)).astype(x.dtype)


def short_gated_conv(h, b_gate, c_gate, conv_w, conv_b):
    L = h.shape[1]
    z = c_gate * h
    zp = jnp.pad(z, ((0, 0), (1, 1), (0, 0)))
    conv = (zp[:, 0:L] * conv_w[0] + zp[:, 1:L + 1] * conv_w[1]
            + zp[:, 2:L + 2] * conv_w[2] + conv_b)
    return b_gate * conv


def multiscale_pool(z, pool_w, pool_scale):
    Bn, L, C = z.shape
    zf = z.astype(jnp.float32).reshape(Bn, L, N_POOL_GROUPS, POOL_GROUP_DIM)
    cs = jnp.concatenate([jnp.zeros_like(zf[:, :1]), jnp.cumsum(zf, axis=1)], axis=1)
    t = np.arange(L)
    avgs = []
    for g, w in enumerate(POOL_WINDOWS):
        hi = np.minimum(t + w // 2, L)
        lo = np.maximum(t - w // 2, 0)
        cs_g = cs[:, :, g]
        s = jnp.take(cs_g, hi, axis=1) - jnp.take(cs_g, lo, axis=1)
        cnt = (hi - lo).astype(np.float32)
        avgs.append(s / cnt[None, :, None])
    avg = jnp.stack(avgs, axis=2)
    mixed = (avg - zf).astype(z.dtype)
    y = jnp.einsum('blgc,gcd->blgd', mixed, pool_w)
    return y.reshape(Bn, L, C) * pool_scale


def peer_ffn(xn, w_q, peer_keys, peer_u, peer_v):
    Bn, L, D = xn.shape
    tok = xn.reshape(-1, D)
    T = tok.shape[0]
    pad = (-T) % PEER_CHUNK
    chunks = jnp.pad(tok, ((0, pad), (0, 0))).reshape(-1, PEER_CHUNK, D)

    def chunk_fn(xc):
        c = xc.shape[0]
        q = (xc @ w_q).reshape(c, PEER_HEADS, 2, PEER_KEY_HALF)
        s = jnp.einsum('chpd,hpnd->chpn', q, peer_keys).astype(jnp.float32)
        sv, si = lax.top_k(s, PEER_TOPK)
        cand = sv[:, :, 0, :, None] + sv[:, :, 1, None, :]
        cand_idx = si[:, :, 0, :, None] * N_KEYS + si[:, :, 1, None, :]
        cand = cand.reshape(c, PEER_HEADS, PEER_TOPK * PEER_TOPK)
        cand_idx = cand_idx.reshape(c, PEER_HEADS, PEER_TOPK * PEER_TOPK)
        top_s, pos = lax.top_k(cand, PEER_TOPK)
        idx = jnp.take_along_axis(cand_idx, pos, axis=-1)
        gate = jax.nn.softmax(top_s, axis=-1)
        u_sel = peer_u[idx]
        v_sel = peer_v[idx]
        act = jax.nn.gelu(jnp.einsum('chkd,cd->chk', u_sel, xc).astype(jnp.float32),
                          approximate=False)
        wts = (gate * act).astype(xc.dtype)
        return jnp.einsum('chk,chkd->cd', wts, v_sel)

    out = lax.map(chunk_fn, chunks)
    return out.reshape(-1, D)[:T].reshape(Bn, L, D)


def setup_inputs(seed: int = 0) -> dict:
    key = jax.random.key(seed)
    ks = jax.random.split(key, 15)
    f32 = jnp.float32
    d = D_MODEL
    return {
        "x": jax.random.normal(ks[0], (BATCH, SEQ, d), f32),
        "meta_tokens": jax.random.normal(ks[1], (N_META, d), f32),
        "norm1_g": 1.0 + 0.02 * jax.random.normal(ks[2], (d,), f32),
        "w_in": jax.random.normal(ks[3], (d, IN_PROJ_COLS), f32) * d ** -0.5,
        "conv_w": jax.random.normal(ks[4], (CONV_WIDTH, CONV_CH), f32) * CONV_WIDTH ** -0.5,
        "conv_b": 0.01 * jax.random.normal(ks[5], (CONV_CH,), f32),
        "pool_w": jax.random.normal(ks[6], (N_POOL_GROUPS, POOL_GROUP_DIM, POOL_GROUP_DIM), f32)
                  * POOL_GROUP_DIM ** -0.5,
        "pool_scale": 1.0 + 0.1 * jax.random.normal(ks[7], (POOL_CH,), f32),
        "w_out": jax.random.normal(ks[8], (MIX_WIDTH, d), f32) * MIX_WIDTH ** -0.5,
        "norm2_g": 1.0 + 0.02 * jax.random.normal(ks[9], (d,), f32),
        "peer_w_q": jax.random.normal(ks[10], (d, PEER_HEADS * PEER_KEY_DIM), f32) * d ** -0.5,
        "peer_keys": jax.random.normal(ks[11], (PEER_HEADS, 2, N_KEYS, PEER_KEY_HALF), f32)
                     * PEER_KEY_HALF ** -0.5,
        "peer_u": jax.random.normal(ks[12], (N_EXPERTS, d), f32) * d ** -0.5,
        "peer_v": jax.random.normal(ks[13], (N_EXPERTS, d), f32) * PEER_HEADS ** -0.5,
        "final_norm_g": 1.0 + 0.02 * jax.random.normal(ks[14], (d,), f32),
    }


def reference(x, meta_tokens, norm1_g, w_in, conv_w, conv_b, pool_w, pool_scale, w_out,
              norm2_g, peer_w_q, peer_keys, peer_u, peer_v, final_norm_g):
    Bn = x.shape[0]
    meta = jnp.broadcast_to(meta_tokens.astype(x.dtype)[None], (Bn, N_META, D_MODEL))
    h = jnp.concatenate([meta, x], axis=1)

    for _ in range(DEPTH):
        hn = rms_norm(h, norm1_g)
        p = hn @ w_in
        hA = p[..., 0:CONV_CH]
        b_gate = p[..., CONV_CH:2 * CONV_CH]
        c_gate = p[..., 2 * CONV_CH:3 * CONV_CH]
        hB = p[..., 3 * CONV_CH:]
        yA = short_gated_conv(hA, b_gate, c_gate, conv_w, conv_b)
        yB = multiscale_pool(hB, pool_w, pool_scale)
        h = h + jnp.concatenate([yA, yB], axis=-1) @ w_out
        h = h + peer_ffn(rms_norm(h, norm2_g), peer_w_q, peer_keys, peer_u, peer_v)

    out = rms_norm(h, final_norm_g)
    return out[:, N_META:]
```

```python
import numpy as np
import concourse.bass as bass
import concourse.mybir as mybir
from concourse.bass_types import AP
from concourse.bass_utils import run_bass_kernel_spmd

F32 = mybir.dt.float32
BF16 = mybir.dt.bfloat16
U32 = mybir.dt.uint32
ALU = mybir.AluOpType
AF = mybir.ActivationFunctionType
AX = mybir.AxisListType

D = 1024
NTOK = 4096
TS = 256
HALO = 8
TW = TS + 2 * HALO
NTILES = NTOK // TS
XW = NTOK + 2 * HALO
EPS = 1e-6
NR = 6
SEM_ROLL = 16000


class Sync:
    def __init__(self, nc, needed=None):
        self.nc = nc
        self.dry = needed is None
        self.needed = set() if self.dry else needed
        self.engs = {'pe': nc.tensor, 'dve': nc.vector, 'act': nc.scalar, 'pool': nc.gpsimd, 'sp': nc.sync}
        self.sems = {}
        self.val = {}
        self.seen = {e: {} for e in self.engs}
        self.res = {}
        self.cur = {}
        self.gen = {e: 0 for e in self.engs}
        self.idx = {e: 0 for e in self.engs}
        self.evval = {}
        for e in self.engs:
            self.cur[e] = 'c_%s_0' % e
            self._sem(self.cur[e])

    def _sem(self, name):
        if name not in self.sems:
            self.sems[name] = None if self.dry else self.nc.alloc_semaphore(name)
            self.val[name] = 0
        return self.sems[name]

    def _wait(self, eng, reads, writes):
        deps = {}

        def add(ev):
            if ev is None:
                return
            key = (ev[0], ev[1])
            if deps.get(key, -1) < ev[2]:
                deps[key] = ev[2]
        for k in reads:
            st = self.res.get(k)
            if st:
                add(st['w'])
        for k in writes:
            st = self.res.get(k)
            if st:
                add(st['w'])
                for r in st['r']:
                    add(r)
        E = self.engs[eng]
        for key, v in deps.items():
            if key[0] == 'c' and eng == 'pe' and key[1] == 'pe':
                continue
            if self.seen[eng].get(key, -1) >= v:
                continue
            self.seen[eng][key] = v
            if key[0] == 'c':
                if self.dry:
                    self.needed.add((key[1], v))
                else:
                    sname, sv = self.evval[(key[1], v)]
                    E.wait_ge(self.sems[sname], sv)
            else:
                if not self.dry:
                    E.wait_ge(self.sems[key[1]], v)

    def _done(self, ev, reads, writes):
        for k in reads:
            st = self.res.setdefault(k, {'w': None, 'r': []})
            st['r'].append(ev)
            if len(st['r']) > 64:
                best = {}
                for e in st['r']:
                    kk = (e[0], e[1])
                    if best.get(kk, -1) < e[2]:
                        best[kk] = e[2]
                st['r'] = [(kk[0], kk[1], v) for kk, v in best.items()]
        for k in writes:
            self.res[k] = {'w': ev, 'r': []}

    def op(self, eng, fn, reads=(), writes=()):
        self._wait(eng, reads, writes)
        ins = fn(self.engs[eng])
        i = self.idx[eng]
        self.idx[eng] += 1
        if (not self.dry) and (eng, i) in self.needed:
            s = self.cur[eng]
            if self.val[s] >= SEM_ROLL:
                self.gen[eng] += 1
                s = 'c_%s_%d' % (eng, self.gen[eng])
                self._sem(s)
                self.cur[eng] = s
            self.val[s] += 1
            ins.then_inc(self.sems[s], 1)
            self.evval[(eng, i)] = (s, self.val[s])
        self._done(('c', eng, i), reads, writes)

    def dma(self, q, out, in_, reads=(), writes=(), sem=None, **kw):
        self._wait(q, reads, writes)
        self._sem(sem)
        ins = self.engs[q].dma_start(out=out, in_=in_, **kw)
        self.val[sem] += 16
        if not self.dry:
            ins.then_inc(self.sems[sem], 16)
        self._done(('d', sem, self.val[sem]), reads, writes)


def build(ntiles=NTILES):
    dry = _build(ntiles, None)
    return _build(ntiles, dry)


def _build(ntiles, needed):
    nc = bass.Bass("TRN2", target_bir_lowering=False)
    xT_d = nc.dram_tensor("xT", [D, XW], F32, kind="ExternalInput").ap()
    invc_d = nc.dram_tensor("invc", [128, 4 * NTOK], F32, kind="ExternalInput").ap()
    vecs_d = nc.dram_tensor("vecs", [128, 44], F32, kind="ExternalInput").ap()
    cst_d = nc.dram_tensor("cst", [128, 256], F32, kind="ExternalInput").ap()
    gf_d = nc.dram_tensor("gfrep", [128, D], F32, kind="ExternalInput").ap()
    win_d = nc.dram_tensor("win", [16, 128, 1024], F32, kind="ExternalInput").ap()
    wout_d = nc.dram_tensor("wout", [8, 128, 1024], F32, kind="ExternalInput").ap()
    wq_d = nc.dram_tensor("wq", [16, 128, 1024], F32, kind="ExternalInput").ap()
    keys_d = nc.dram_tensor("keys", [2, 128, 1024], F32, kind="ExternalInput").ap()
    poolw_d = nc.dram_tensor("poolw", [128, 512], F32, kind="ExternalInput").ap()
    u_d = nc.dram_tensor("u", [128, 128, 1024], F32, kind="ExternalInput").ap()
    v_d = nc.dram_tensor("v", [128, 128, 1024], F32, kind="ExternalInput").ap()
    out_d = nc.dram_tensor("out", [NTOK, D], F32, kind="ExternalOutput").ap()
    uvbf_d = nc.dram_tensor("uvbf", [128, 128, 2048], BF16, kind="Internal").ap()
    fwbf_d = nc.dram_tensor("fwbf", [40, 128, 1024], BF16, kind="Internal").ap()

    S = Sync(nc, needed)
    A = nc.alloc_sbuf_tensor
    hT = A("hT", [128, 8, TW], F32)
    rstd = A("rstd", [128, TW], F32)
    big = A("big", [128, 16 * TW], F32)
    zt = A("zt", [128, TW], F32)
    t1 = A("t1", [128, TW], F32)
    sA = A("sA", [128, TW], F32)
    sB = A("sB", [128, TW], F32)
    mixed = A("mixed", [128, TS], BF16)
    yT = A("yT", [128, 8, TS], BF16)
    h1T = [A("h1T%d" % i, [128, 8, TS], F32) for i in range(2)]
    xn2 = [A("xn2_%d" % i, [128, 8, TS], BF16) for i in range(2)]
    qT = A("qT", [128, 16, TS], BF16)
    wr = [A("wr%d" % i, [128, 2048], BF16) for i in range(NR)]
    fw = [A("fw%d" % i, [128, 1024], BF16) for i in range(3)]
    keysT = A("keysT", [128, 2048], BF16)
    poolw = A("poolw_sb", [128, 512], BF16)
    vecs = A("vecs_sb", [128, 44], F32)
    cst = A("cst_sb", [128, 256], F32)
    gfrep = A("gfrep_sb", [128, D], F32)
    ones = A("ones", [128, 128], F32)
    iota_bf = A("iota_bf", [128, 128], BF16)
    invc = A("invc_sb", [128, 4, TS], F32)
    sv = A("sv", [128, 16, 16], F32)
    si = A("si", [128, 16, 16], U32)
    sif = A("sif", [128, 16, 16], F32)
    cand = A("cand", [128, 8, 256], F32)
    tops = A("tops", [128, 8, 16], F32)
    pos = A("pos", [128, 8, 16], U32)
    au = A("au", [128, 8, 16], U32)
    bu = A("bu", [128, 8, 16], U32)
    af = A("af", [128, 8, 16], F32)
    bf = A("bf", [128, 8, 16], F32)
    ef = A("ef", [128, 8, 16], F32)
    Zt = A("Zt", [128, 8], F32)
    gate = A("gate", [128, 8, 16], F32)
    i0f = A("i0f", [128, 8, 16], F32)
    i1f = A("i1f", [128, 8, 16], F32)
    i0T = A("i0T", [128, TS], F32)
    i1T = A("i1T", [128, TS], F32)
    gT = A("gT", [128, TS], F32)
    GT = 8
    P1oh = [A("P1oh%d" % i, [128, GT, 128], BF16) for i in range(2)]
    Q0oh = [A("Q0oh%d" % i, [128, GT, 128], BF16) for i in range(2)]
    WG = A("WG", [128, 128, TS], BF16)
    Hb = [A("Hb%d" % i, [128, TS], BF16) for i in range(3)]
    Gb = [A("Gb%d" % i, [128, TS], BF16) for i in range(4)]
    ssq = A("ssq", [128, 2], F32)
    ps = [nc.alloc_psum_tensor("ps%d" % i, [128, 512], F32) for i in range(8)]

    ident = cst[:, 0:128]
    ot = big[:, 0:1024]
    h1tok = big[:, 1024:2048]
    hn = qT[:].rearrange("p c t -> p (c t)")[:, 0:8 * TW].rearrange("p (c t) -> p c t", c=8)
    iota = cst[:, 128:256]
    pT = big[:, 0:16 * TW].rearrange("p (c t) -> p c t", c=16)
    sqv = big[:, 0:8 * TW].rearrange("p (c t) -> p c t", c=8)
    Ssb = big[:, 0:2048].rearrange("p (a b) -> p a b", a=16)
    S2 = big[:, 2048:4096].rearrange("p (a b) -> p a b", a=16)
    cand2 = big[:, 0:2048].rearrange("p (a b) -> p a b", a=8)
    eq = big[:, 2048:4096].rearrange("p (h k a) -> p h k a", h=8, k=16)

    def pstride(t):
        return list(t[:].ap[0])

    state = {'fb': 0, 'wb': 0, 'wk': 0, 'fk': 0}
    FB = (6, 7)
    WB = (0, 1)

    def fbank():
        b = FB[state['fb'] % 2]
        state['fb'] += 1
        return b

    def wbank():
        b = WB[state['wb'] % 2]
        state['wb'] += 1
        return b

    pref = {}

    def fsrc(idx):
        if idx < 16:
            return win_d[idx]
        if idx < 24:
            return wout_d[idx - 16]
        return wq_d[idx - 24]

    def fissue(ti, idx):
        k = state['fk'] % 3
        state['fk'] += 1
        cres = 'fwbf%d' % idx
        if ti == 0:
            S.dma('pool', fw[k][:], fsrc(idx), writes=['fw%d' % k], sem='d_fw%d' % k)
            S.dma('sp', fwbf_d[idx], fw[k][:], reads=['fw%d' % k], writes=[cres], sem='d_fsb%d' % k)
        else:
            S.dma('sp', fw[k][:], fwbf_d[idx], reads=[cres], writes=['fw%d' % k], sem='d_fwh%d' % k)
        pref[(ti, idx)] = k

    def fload(src, ti, idx):
        if (ti, idx) not in pref:
            fissue(ti, idx)
        k = pref.pop((ti, idx))
        for nxt in (idx + 1, idx + 2):
            if nxt < 40 and (ti, nxt) not in pref:
                fissue(ti, nxt)
        return k

    S.dma('sp', vecs[:], vecs_d, writes=['vecs'], sem='d_vecs')
    S.dma('sp', cst[:], cst_d, writes=['cst'], sem='d_cst')
    S.dma('sp', gfrep[:], gf_d, writes=['gfrep'], sem='d_gf')
    S.dma('pool', keysT[:, 0:1024], keys_d[0], writes=['keysT'], sem='d_k')
    S.dma('pool', keysT[:, 1024:2048], keys_d[1], writes=['keysT'], sem='d_k')
    S.dma('pool', poolw[:], poolw_d, writes=['poolw'], sem='d_pw')
    S.op('dve', lambda e: e.memset(ones[:], 1.0), writes=['ones'])
    S.op('dve', lambda e: e.tensor_copy(iota_bf[:], cst[:, 128:256]), reads=['cst'], writes=['iota_bf'])

    xT_v = xT_d.rearrange("(c p) t -> p c t", p=128)
    invc_v = invc_d.rearrange("p (g t) -> p g t", g=4)

    def rmsnorm_units(units, src, srcres, W, gcol, dst, dstres):
        def u_sq():
            S.op('act', lambda e: e.activation(sqv[:, :, 0:W], src[:], AF.Square), reads=[srcres], writes=['big'])
        def u_ssq():
            b = fbank()
            for dc in range(8):
                S.op('pe', lambda e: e.matmul(ps[b][:, 0:W], ones[:], sqv[:, dc, 0:W], start=(dc == 0), stop=(dc == 7)),
                     reads=['ones', 'big'], writes=['ps%d' % b])
            S.op('act', lambda e: e.activation(rstd[:, 0:W], ps[b][:, 0:W], AF.Sqrt, bias=EPS, scale=1.0 / D),
                 reads=['ps%d' % b], writes=['rstd'])
            S.op('dve', lambda e: e.reciprocal(rstd[:, 0:W], rstd[:, 0:W]), reads=['rstd'], writes=['rstd'])
        def u_scale(dc0):
            for dc in range(dc0, dc0 + 4):
                S.op('dve', lambda e: e.scalar_tensor_tensor(dst[:, dc, :], src[:, dc, :], vecs[:, gcol + dc:gcol + dc + 1],
                                                            rstd[:, 0:W], ALU.mult, ALU.mult),
                     reads=[srcres, 'rstd', 'vecs'], writes=[dstres])
        units.append(u_sq)
        units.append(u_ssq)
        units.append(lambda: u_scale(0))
        units.append(lambda: u_scale(4))

    def front_units(ti):
        par = ti % 2
        t0 = ti * TS
        h1 = h1T[par]
        xn = xn2[par]
        h1res = 'h1T%d' % par
        xnres = 'xn2_%d' % par
        units = []

        def u_load():
            S.dma('sp', hT[:], xT_v[:, :, t0:t0 + TW], writes=['hT'], sem='d_x')
            S.dma('sp', invc[:], invc_v[:, :, t0:t0 + TS], writes=['invc'], sem='d_i')
        units.append(u_load)
        rmsnorm_units(units, hT, 'hT', TW, 0, hn, 'qT')

        def u_inproj(cc):
            k = fload(win_d[cc], ti, cc)
            b = fbank()
            for dc in range(8):
                S.op('pe', lambda e: e.matmul(ps[b][:, 0:TW], fw[k][:, dc * 128:(dc + 1) * 128], hn[:, dc, :],
                                              start=(dc == 0), stop=(dc == 7)),
                     reads=['fw%d' % k, 'qT'], writes=['ps%d' % b])
            S.op('act', lambda e: e.copy(pT[:, cc, :], ps[b][:, 0:TW]), reads=['ps%d' % b], writes=['big'])
        for cc in range(16):
            units.append(lambda cc=cc: u_inproj(cc))

        def u_conv(c):
            S.op('dve', lambda e: e.tensor_tensor(zt[:], pT[:, 8 + c, :], pT[:, c, :], ALU.mult), reads=['big'], writes=['zt'])
            S.op('dve', lambda e: e.tensor_scalar(t1[:, 0:TS], zt[:, 7:7 + TS], vecs[:, 24 + c:25 + c], None, ALU.mult),
                 reads=['zt', 'vecs'], writes=['t1'])
            S.op('dve', lambda e: e.scalar_tensor_tensor(t1[:, 0:TS], zt[:, 8:8 + TS], vecs[:, 28 + c:29 + c], t1[:, 0:TS], ALU.mult, ALU.add),
                 reads=['zt', 'vecs', 't1'], writes=['t1'])
            S.op('dve', lambda e: e.scalar_tensor_tensor(t1[:, 0:TS], zt[:, 9:9 + TS], vecs[:, 32 + c:33 + c], t1[:, 0:TS], ALU.mult, ALU.add),
                 reads=['zt', 'vecs', 't1'], writes=['t1'])
            S.op('dve', lambda e: e.scalar_tensor_tensor(yT[:, c, :], t1[:, 0:TS], vecs[:, 36 + c:37 + c], pT[:, 4 + c, 8:8 + TS], ALU.add, ALU.mult),
                 reads=['t1', 'vecs', 'big'], writes=['yT'])
        for c in range(4):
            units.append(lambda c=c: u_conv(c))

        def u_pool(g, w):
            x = pT[:, 12 + g, :]
            S.op('dve', lambda e: e.tensor_tensor(sA[:, 1:272], x[:, 0:271], x[:, 1:272], ALU.add), reads=['big'], writes=['sA'])
            fin = sA
            if w >= 4:
                S.op('dve', lambda e: e.tensor_tensor(sB[:, 2:271], sA[:, 1:270], sA[:, 3:272], ALU.add), reads=['sA'], writes=['sB'])
                fin = sB
            if w >= 8:
                S.op('dve', lambda e: e.tensor_tensor(sA[:, 4:269], sB[:, 2:267], sB[:, 6:271], ALU.add), reads=['sB'], writes=['sA'])
                fin = sA
            if w >= 16:
                S.op('dve', lambda e: e.tensor_tensor(sB[:, 8:265], sA[:, 4:261], sA[:, 12:269], ALU.add), reads=['sA'], writes=['sB'])
                fin = sB
            S.op('dve', lambda e: e.tensor_tensor(zt[:, 0:TS], fin[:, 8:8 + TS], invc[:, g, :], ALU.mult),
                 reads=['sA', 'sB', 'invc'], writes=['zt'])
            S.op('dve', lambda e: e.tensor_tensor(mixed[:], zt[:, 0:TS], x[:, 8:8 + TS], ALU.subtract),
                 reads=['zt', 'big'], writes=['mixed'])
            b = fbank()
            S.op('pe', lambda e: e.matmul(ps[b][:, 0:TS], poolw[:, g * 128:(g + 1) * 128], mixed[:], start=True, stop=True),
                 reads=['poolw', 'mixed'], writes=['ps%d' % b])
            S.op('dve', lambda e: e.tensor_scalar(yT[:, 4 + g, :], ps[b][:, 0:TS], vecs[:, 40 + g:41 + g], None, ALU.mult),
                 reads=['ps%d' % b, 'vecs'], writes=['yT'])
        for g, w in enumerate((2, 4, 8, 16)):
            units.append(lambda g=g, w=w: u_pool(g, w))

        def u_outproj(dd):
            k = fload(wout_d[dd], ti, 16 + dd)
            b = fbank()
            for cc in range(8):
                S.op('pe', lambda e: e.matmul(ps[b][:, 0:TS], fw[k][:, cc * 128:(cc + 1) * 128], yT[:, cc, :],
                                              start=(cc == 0), stop=(cc == 7)),
                     reads=['fw%d' % k, 'yT'], writes=['ps%d' % b])
            S.op('dve', lambda e: e.tensor_tensor(h1[:, dd, :], hT[:, dd, 8:8 + TS], ps[b][:, 0:TS], ALU.add),
                 reads=['hT', 'ps%d' % b], writes=[h1res])
        for dd in range(8):
            units.append(lambda dd=dd: u_outproj(dd))
        rmsnorm_units(units, h1, h1res, TS, 8, xn, xnres)

        def u_q(cc):
            k = fload(wq_d[cc], ti, 24 + cc)
            b = fbank()
            for dc in range(8):
                S.op('pe', lambda e: e.matmul(ps[b][:, 0:TS], fw[k][:, dc * 128:(dc + 1) * 128], xn[:, dc, :],
                                              start=(dc == 0), stop=(dc == 7)),
                     reads=['fw%d' % k, xnres], writes=['ps%d' % b])
            S.op('act', lambda e: e.copy(qT[:, cc, :], ps[b][:, 0:TS]), reads=['ps%d' % b], writes=['qT'])
        for cc in range(16):
            units.append(lambda cc=cc: u_q(cc))

        ts_units = [[], []]
        tr_units = [None, None]
        for ts in range(2):
            tsl = slice(ts * 128, (ts + 1) * 128)
            units_ts = ts_units[ts]

            def u_scores(kq, tsl=tsl):
                b = fbank()
                for c4 in range(4):
                    cc = kq * 4 + c4
                    S.op('pe', lambda e: e.matmul(ps[b][:, c4 * 128:(c4 + 1) * 128], qT[:, cc, tsl],
                                                  keysT[:, cc * 128:(cc + 1) * 128], start=True, stop=True),
                         reads=['qT', 'keysT'], writes=['ps%d' % b])
                S.op('act', lambda e: e.copy(big[:, kq * 512:(kq + 1) * 512], ps[b][:]), reads=['ps%d' % b], writes=['big'])
            for kq in range(4):
                units_ts.append(lambda kq=kq, f=u_scores: f(kq))

            def u_topk(cc):
                S.op('dve', lambda e: e.max(sv[:, cc, 0:8], Ssb[:, cc, :]), reads=['big'], writes=['sv'])
                S.op('dve', lambda e: e.max_index(si[:, cc, 0:8], sv[:, cc, 0:8], Ssb[:, cc, :]), reads=['big', 'sv'], writes=['si'])
                S.op('dve', lambda e: e.match_replace(S2[:, cc, :], sv[:, cc, 0:8], Ssb[:, cc, :], -1e30), reads=['sv'], writes=['big'])
                S.op('dve', lambda e: e.max(sv[:, cc, 8:16], S2[:, cc, :]), reads=['big'], writes=['sv'])
                S.op('dve', lambda e: e.max_index(si[:, cc, 8:16], sv[:, cc, 8:16], S2[:, cc, :]), reads=['big', 'sv'], writes=['si'])
            for cc in range(16):
                units_ts.append(lambda cc=cc: u_topk(cc))

            def u_cand(hh):
                if hh == 0:
                    S.op('dve', lambda e: e.tensor_copy(sif[:], si[:]), reads=['si'], writes=['sif'])
                pp = pstride(sv)
                c_in0 = AP(tensor=sv, offset=hh * 128, ap=[pp, [32, 4], [1, 16], [0, 16]])
                c_in1 = AP(tensor=sv, offset=hh * 128 + 16, ap=[pp, [32, 4], [0, 16], [1, 16]])
                S.op('dve', lambda e: e.tensor_tensor(cand[:, hh * 4:hh * 4 + 4, :].rearrange("p h (a b) -> p h a b", a=16), c_in0, c_in1, ALU.add),
                     reads=['sv'], writes=['cand'])
            units_ts.append(lambda: u_cand(0))
            units_ts.append(lambda: u_cand(1))

            def u_top2(h):
                S.op('dve', lambda e: e.max(tops[:, h, 0:8], cand[:, h, :]), reads=['cand'], writes=['tops'])
                S.op('dve', lambda e: e.max_index(pos[:, h, 0:8], tops[:, h, 0:8], cand[:, h, :]), reads=['cand', 'tops'], writes=['pos'])
                S.op('dve', lambda e: e.match_replace(cand2[:, h, :], tops[:, h, 0:8], cand[:, h, :], -1e30), reads=['cand', 'tops'], writes=['big'])
                S.op('dve', lambda e: e.max(tops[:, h, 8:16], cand2[:, h, :]), reads=['big'], writes=['tops'])
                S.op('dve', lambda e: e.max_index(pos[:, h, 8:16], tops[:, h, 8:16], cand2[:, h, :]), reads=['big', 'tops'], writes=['pos'])
            for h in range(8):
                units_ts.append(lambda h=h: u_top2(h))

            def u_gates(part):
                if part == 0:
                    mx = AP(tensor=tops, offset=0, ap=[pstride(tops), [16, 8], [0, 16]])
                    S.op('dve', lambda e: e.tensor_tensor(ef[:], tops[:], mx, ALU.subtract), reads=['tops'], writes=['ef'])
                    S.op('act', lambda e: e.activation(ef[:], ef[:], AF.Exp), reads=['ef'], writes=['ef'])
                    S.op('dve', lambda e: e.tensor_single_scalar(au[:], pos[:], 4, op=ALU.logical_shift_right), reads=['pos'], writes=['au'])
                    S.op('dve', lambda e: e.tensor_single_scalar(bu[:], pos[:], 15, op=ALU.bitwise_and), reads=['pos'], writes=['bu'])
                    S.op('dve', lambda e: e.tensor_copy(af[:], au[:]), reads=['au'], writes=['af'])
                    S.op('dve', lambda e: e.tensor_copy(bf[:], bu[:]), reads=['bu'], writes=['bf'])
                else:
                    S.op('dve', lambda e: e.tensor_reduce(Zt[:], ef[:], AX.X, ALU.add), reads=['ef'], writes=['Zt'])
                    S.op('dve', lambda e: e.reciprocal(Zt[:], Zt[:]), reads=['Zt'], writes=['Zt'])
                    S.op('dve', lambda e: e.tensor_tensor(gate[:], ef[:], Zt[:].unsqueeze(2).to_broadcast([128, 8, 16]), ALU.mult),
                         reads=['ef', 'Zt'], writes=['gate'])
            units_ts.append(lambda: u_gates(0))
            units_ts.append(lambda: u_gates(1))

            def u_idx(xf, off, dst, hh, stage):
                hs = slice(hh * 4, hh * 4 + 4)
                eqh = eq[:, hs]
                if stage == 0:
                    io16 = AP(tensor=cst, offset=128, ap=[pstride(cst), [0, 4], [0, 16], [1, 16]])
                    xb = AP(tensor=xf, offset=hh * 64, ap=[pstride(xf), [16, 4], [1, 16], [0, 16]])
                    S.op('dve', lambda e: e.tensor_tensor(eqh, io16, xb, ALU.is_equal), reads=['cst', 'af', 'bf'], writes=['big'])
                elif stage == 1:
                    sb = AP(tensor=sif, offset=off + hh * 128, ap=[pstride(sif), [32, 4], [0, 16], [1, 16]])
                    S.op('dve', lambda e: e.tensor_tensor(eqh, eqh, sb, ALU.mult), reads=['sif'], writes=['big'])
                else:
                    S.op('dve', lambda e: e.tensor_reduce(dst[:, hs, :], eqh, AX.X, ALU.add), reads=['big'], writes=[dst.name])
            for (xf_, off_, dst_) in ((af, 0, i0f), (bf, 16, i1f)):
                for hh_ in range(2):
                    for st_ in range(3):
                        units_ts.append(lambda a=xf_, b=off_, c=dst_, d=hh_, f=st_: u_idx(a, b, c, d, f))

            def u_tr(tsl=tsl):
                b = fbank()
                for n, src in enumerate((i0f, i1f, gate)):
                    S.op('pe', lambda e: e.transpose(ps[b][:, n * 128:(n + 1) * 128], src[:].rearrange("p h k -> p (h k)"), ident),
                         reads=[src.name, 'cst'], writes=['ps%d' % b])
                for n, dst in enumerate((i0T, i1T, gT)):
                    S.op('act', lambda e: e.copy(dst[:, tsl], ps[b][:, n * 128:(n + 1) * 128]),
                         reads=['ps%d' % b], writes=[dst.name])
            tr_units[ts] = u_tr
        ts0, ts1 = ts_units
        merged = units + ts0 + ts1[:8] + [tr_units[0]] + ts1[8:] + [tr_units[1]]
        return merged[:21], merged[21:]

    def wbuild(ti, side=()):
        side = list(side)
        for gq in range(TS // GT):
            tq = gq * GT
            bi = gq % 2
            P1, Q0 = P1oh[bi], Q0oh[bi]
            tag = '%d_%d' % (ti, gq)
            for t in range(GT):
                tok = tq + t
                wx = ['ohg%d' % bi] if t == 0 else []
                S.op('dve', lambda e: e.tensor_scalar(P1[:, t, :], iota_bf[:], i1T[:, tok:tok + 1], None, ALU.is_equal),
                     reads=['iota_bf', 'i1T'], writes=wx)
                S.op('dve', lambda e: e.tensor_scalar(Q0[:, t, :], iota_bf[:], i0T[:, tok:tok + 1], gT[:, tok:tok + 1], ALU.is_equal, ALU.mult),
                     reads=['iota_bf', 'i0T', 'gT'], writes=['oh_%s_%d' % (tag, t)])
            for q4 in range(GT // 4):
                b = wbank()
                quad = ['oh_%s_%d' % (tag, q4 * 4 + tt) for tt in range(4)]
                for tt in range(4):
                    t = q4 * 4 + tt
                    S.op('pe', lambda e: e.matmul(ps[b][:].rearrange("p (i t) -> p t i", t=4)[:, tt, :], Q0[:, t, :], P1[:, t, :], start=True, stop=True),
                         reads=quad + ['ohg%d' % bi], writes=['ps%d' % b])
                tg = tq + q4 * 4
                S.op('act', lambda e: e.copy(WG[:, :, tg:tg + 4], ps[b][:].rearrange("p (i t) -> p i t", t=4)),
                     reads=['ps%d' % b], writes=['WGw'])
            if side:
                side.pop(0)()
        while side:
            side.pop(0)()

    def dense(ti):
        par = ti % 2
        xn = xn2[par]
        xnres = 'xn2_%d' % par
        SKEW = 2
        slots = {}

        def load(i1):
            k = state['wk'] % NR
            state['wk'] += 1
            cres = 'uvbf%d' % i1
            if ti == 0:
                S.dma('pool', wr[k][:, 0:1024], u_d[i1], writes=['wr%d' % k], sem='d_wr%d' % k)
                S.dma('pool', wr[k][:, 1024:2048], v_d[i1], writes=['wr%d' % k], sem='d_wr%d' % k)
                S.dma('sp', uvbf_d[i1], wr[k][:], reads=['wr%d' % k], writes=[cres], sem='d_sb%d' % k)
            else:
                S.dma('sp', wr[k][:], uvbf_d[i1], reads=[cres], writes=['wr%d' % k], sem='d_wrh%d' % k)
            slots[i1] = k
            return k

        for step in range(128 + SKEW):
            i1 = step
            if i1 < 128:
                k = load(i1)
                b = i1 % 2
                for dc in range(8):
                    S.op('pe', lambda e: e.matmul(ps[b][:, 0:TS], wr[k][:, dc * 128:(dc + 1) * 128], xn[:, dc, :],
                                                  start=(dc == 0), stop=(dc == 7)),
                         reads=['wr%d' % k, xnres], writes=['ps%d' % b])
                hb = Hb[i1 % 3]
                gbuf = Gb[i1 % 4]
                S.op('act', lambda e: e.activation(hb[:], ps[b][:, 0:TS], AF.Gelu), reads=['ps%d' % b], writes=['Hb%d' % (i1 % 3)])
                S.op('dve', lambda e: e.tensor_tensor(gbuf[:], hb[:], WG[:, i1, :], ALU.mult),
                     reads=['Hb%d' % (i1 % 3), 'WGw'], writes=['Gb%d' % (i1 % 4)])
            j1 = step - SKEW
            if j1 >= 0:
                k = slots.pop(j1)
                gbuf = Gb[j1 % 4]
                for ts in range(2):
                    for dh in range(2):
                        bk = 2 + ts * 2 + dh
                        S.op('pe', lambda e: e.matmul(ps[bk][:], gbuf[:, ts * 128:(ts + 1) * 128], wr[k][:, 1024 + dh * 512:1024 + (dh + 1) * 512],
                                                      start=(j1 == 0), stop=(j1 == 127)),
                             reads=['wr%d' % k, 'Gb%d' % (j1 % 4)], writes=['ps%d' % bk])
            yield

    def final_units(ti):
        par = ti % 2
        h1 = h1T[par]
        h1res = 'h1T%d' % par
        t0 = ti * TS
        units = []

        def u_tp(ts, hf):
            b = fbank()
            for c4 in range(4):
                dc = hf * 4 + c4
                S.op('pe', lambda e: e.transpose(ps[b][:, c4 * 128:(c4 + 1) * 128], h1[:, dc, ts * 128:(ts + 1) * 128], ident),
                     reads=[h1res, 'cst'], writes=['ps%d' % b])
            S.op('act', lambda e: e.copy(h1tok[:, hf * 512:(hf + 1) * 512], ps[b][:]), reads=['ps%d' % b], writes=['big'])

        def u_add(ts):
            for dh in range(2):
                bk = 2 + ts * 2 + dh
                S.op('dve', lambda e: e.tensor_tensor(h1tok[:, dh * 512:(dh + 1) * 512], h1tok[:, dh * 512:(dh + 1) * 512], ps[bk][:], ALU.add),
                     reads=['ps%d' % bk], writes=['big'])

        def u_norm(ts):
            S.op('act', lambda e: e.activation(ot, h1tok, AF.Square, accum_out=ssq[:, 0:1]), reads=[], writes=['big', 'ssq'])
            S.op('act', lambda e: e.activation(ssq[:, 1:2], ssq[:, 0:1], AF.Sqrt, bias=EPS, scale=1.0 / D), reads=['ssq'], writes=['ssq'])
            S.op('dve', lambda e: e.reciprocal(ssq[:, 1:2], ssq[:, 1:2]), reads=['ssq'], writes=['ssq'])
            S.op('dve', lambda e: e.scalar_tensor_tensor(ot, h1tok, ssq[:, 1:2], gfrep[:], ALU.mult, ALU.mult),
                 reads=['ssq', 'gfrep'], writes=['big'])
            r0 = t0 + ts * 128
            S.dma('sp', out_d[r0:r0 + 128, :], ot, reads=['big'], sem='d_out')

        for ts in range(2):
            units.append(lambda ts=ts: u_tp(ts, 0))
            units.append(lambda ts=ts: u_tp(ts, 1))
            units.append(lambda ts=ts: u_add(ts))
            units.append(lambda ts=ts: u_norm(ts))
        return units

    fA, fB = front_units(0)
    for u in fA + fB:
        u()
    nA, nB = front_units(1) if ntiles > 1 else ([], [])
    wbuild(0, nA)
    for ti in range(ntiles):
        side = nB
        nside = len(side)
        done = 0
        nsteps = 131
        cost = 0.0
        per_step = state.get('side_cost', 650.0) / float(nsteps - 14)
        for step, _ in enumerate(dense(ti)):
            while done < nside and cost < (step + 1) * per_step:
                a_d, a_p = S.idx['dve'], S.idx['pe']
                side[done]()
                done += 1
                cost += (S.idx['dve'] - a_d) + 0.2 * (S.idx['pe'] - a_p)
        while done < nside:
            a_d, a_p = S.idx['dve'], S.idx['pe']
            side[done]()
            done += 1
            cost += (S.idx['dve'] - a_d) + 0.2 * (S.idx['pe'] - a_p)
        if nside:
            state['side_cost'] = cost
        if ti + 1 < ntiles:
            nA, nB = front_units(ti + 2) if ti + 2 < ntiles else ([], [])
            wbuild(ti + 1, final_units(ti) + nA)
        else:
            for u in final_units(ti):
                u()

    if S.dry:
        return S.needed
    nc.sync.wait_ge(S.sems['d_out'], S.val['d_out'])
    return nc


def _prep_weights(inputs):
    f = lambda a: np.ascontiguousarray(np.asarray(a, dtype=np.float32))
    w_in = f(inputs["w_in"])
    win = w_in.reshape(8, 128, 16, 128).transpose(2, 1, 0, 3).reshape(16, 128, 1024)
    w_out = f(inputs["w_out"])
    wout = w_out.reshape(8, 128, 8, 128).transpose(2, 1, 0, 3).reshape(8, 128, 1024)
    w_q = f(inputs["peer_w_q"])
    wq = w_q.reshape(8, 128, 16, 128).transpose(2, 1, 0, 3).reshape(16, 128, 1024)
    keys = f(inputs["peer_keys"]).reshape(16, 128, 128)
    keys_l = keys.transpose(2, 0, 1).reshape(128, 2, 1024).transpose(1, 0, 2)
    poolw = f(inputs["pool_w"]).transpose(1, 0, 2).reshape(128, 512)
    u = f(inputs["peer_u"]).reshape(128, 128, 8, 128)
    u_l = u.transpose(1, 3, 2, 0).reshape(128, 128, 1024)
    v = f(inputs["peer_v"]).reshape(128, 128, 1024)
    v_l = v.transpose(1, 0, 2)
    vecs = np.zeros((128, 44), np.float32)
    vecs[:, 0:8] = f(inputs["norm1_g"]).reshape(8, 128).T
    vecs[:, 8:16] = f(inputs["norm2_g"]).reshape(8, 128).T
    vecs[:, 16:24] = f(inputs["final_norm_g"]).reshape(8, 128).T
    cw = f(inputs["conv_w"])
    for kk in range(3):
        vecs[:, 24 + 4 * kk:28 + 4 * kk] = cw[kk].reshape(4, 128).T
    vecs[:, 36:40] = f(inputs["conv_b"]).reshape(4, 128).T
    vecs[:, 40:44] = f(inputs["pool_scale"]).reshape(4, 128).T
    cst = np.zeros((128, 256), np.float32)
    cst[:, 0:128] = np.eye(128, dtype=np.float32)
    cst[:, 128:256] = np.arange(128, dtype=np.float32)[None, :]
    c = np.ascontiguousarray
    gfrep = np.ascontiguousarray(np.broadcast_to(f(inputs["final_norm_g"]).reshape(1, D), (128, D)))
    return dict(win=c(win), wout=c(wout), wq=c(wq), keys=c(keys_l), poolw=c(poolw), u=c(u_l), v=c(v_l),
                vecs=vecs, cst=cst, gfrep=gfrep)


def _prep_core(inputs, b, half):
    x = np.asarray(inputs["x"], dtype=np.float32)
    meta = np.asarray(inputs["meta_tokens"], dtype=np.float32)
    L = 16 + x.shape[1]
    p0 = 16 + NTOK * half
    lo, hi = p0 - HALO, p0 + NTOK + HALO
    rows = np.zeros((XW, D), np.float32)
    full_lo = lo
    for (a, bnd) in ((lo, min(hi, 16)),):
        if a < bnd:
            rows[a - full_lo:bnd - full_lo] = meta[a:bnd]
    a = max(lo, 16)
    bnd = min(hi, L)
    rows[a - full_lo:bnd - full_lo] = x[b, a - 16:bnd - 16]
    xT = np.ascontiguousarray(rows.T)
    p = p0 + np.arange(NTOK)
    inv = np.zeros((4, NTOK), np.float32)
    for g, w in enumerate((2, 4, 8, 16)):
        cnt = np.minimum(p + w // 2, L) - np.maximum(p - w // 2, 0)
        inv[g] = 1.0 / cnt.astype(np.float32)
    invc = np.ascontiguousarray(np.broadcast_to(inv.reshape(1, 4 * NTOK), (128, 4 * NTOK)))
    return dict(xT=xT, invc=invc)


def kernel(**inputs):
    wts = _prep_weights(inputs)
    in_maps = []
    for core in range(8):
        b, half = core // 2, core % 2
        m = dict(wts)
        m.update(_prep_core(inputs, b, half))
        in_maps.append(m)
    nc = build()
    res = run_bass_kernel_spmd(nc, in_maps, core_ids=list(range(8)))
    B = 4
    out = np.empty((B, 2 * NTOK, D), np.float32)
    for core in range(8):
        b, half = core // 2, core % 2
        out[b, half * NTOK:(half + 1) * NTOK, :] = res.results[core]["out"]
    return out
```

```python
import numpy as np
import concourse.bass as bass
import concourse.mybir as mybir
from concourse.bass_types import AP
from concourse.bass_utils import run_bass_kernel_spmd

F32 = mybir.dt.float32
BF16 = mybir.dt.bfloat16
U32 = mybir.dt.uint32
ALU = mybir.AluOpType
AF = mybir.ActivationFunctionType
AX = mybir.AxisListType

D = 1024
NTOK = 4096
TS = 256
HALO = 8
TW = TS + 2 * HALO
NTILES = NTOK // TS
XW = NTOK + 2 * HALO
EPS = 1e-6
NR = 6
SEM_ROLL = 16000


class Sync:
    def __init__(self, nc, needed=None):
        self.nc = nc
        self.dry = needed is None
        self.needed = set() if self.dry else needed
        self.engs = {'pe': nc.tensor, 'dve': nc.vector, 'act': nc.scalar, 'pool': nc.gpsimd, 'sp': nc.sync}
        self.sems = {}
        self.val = {}
        self.seen = {e: {} for e in self.engs}
        self.res = {}
        self.cur = {}
        self.gen = {e: 0 for e in self.engs}
        self.idx = {e: 0 for e in self.engs}
        self.evval = {}
        for e in self.engs:
            self.cur[e] = 'c_%s_0' % e
            self._sem(self.cur[e])

    def _sem(self, name):
        if name not in self.sems:
            self.sems[name] = None if self.dry else self.nc.alloc_semaphore(name)
            self.val[name] = 0
        return self.sems[name]

    def _wait(self, eng, reads, writes):
        deps = {}

        def add(ev):
            if ev is None:
                return
            key = (ev[0], ev[1])
            if deps.get(key, -1) < ev[2]:
                deps[key] = ev[2]
        for k in reads:
            st = self.res.get(k)
            if st:
                add(st['w'])
        for k in writes:
            st = self.res.get(k)
            if st:
                add(st['w'])
                for r in st['r']:
                    add(r)
        E = self.engs[eng]
        for key, v in deps.items():
            if key[0] == 'c' and eng == 'pe' and key[1] == 'pe':
                continue
            if self.seen[eng].get(key, -1) >= v:
                continue
            self.seen[eng][key] = v
            if key[0] == 'c':
                if self.dry:
                    self.needed.add((key[1], v))
                else:
                    sname, sv = self.evval[(key[1], v)]
                    E.wait_ge(self.sems[sname], sv)
            else:
                if not self.dry:
                    E.wait_ge(self.sems[key[1]], v)

    def _done(self, ev, reads, writes):
        for k in reads:
            st = self.res.setdefault(k, {'w': None, 'r': []})
            st['r'].append(ev)
            if len(st['r']) > 64:
                best = {}
                for e in st['r']:
                    kk = (e[0], e[1])
                    if best.get(kk, -1) < e[2]:
                        best[kk] = e[2]
                st['r'] = [(kk[0], kk[1], v) for kk, v in best.items()]
        for k in writes:
            self.res[k] = {'w': ev, 'r': []}

    def op(self, eng, fn, reads=(), writes=()):
        self._wait(eng, reads, writes)
        ins = fn(self.engs[eng])
        i = self.idx[eng]
        self.idx[eng] += 1
        if (not self.dry) and (eng, i) in self.needed:
            s = self.cur[eng]
            if self.val[s] >= SEM_ROLL:
                self.gen[eng] += 1
                s = 'c_%s_%d' % (eng, self.gen[eng])
                self._sem(s)
                self.cur[eng] = s
            self.val[s] += 1
            ins.then_inc(self.sems[s], 1)
            self.evval[(eng, i)] = (s, self.val[s])
        self._done(('c', eng, i), reads, writes)

    def dma(self, q, out, in_, reads=(), writes=(), sem=None, **kw):
        self._wait(q, reads, writes)
        self._sem(sem)
        ins = self.engs[q].dma_start(out=out, in_=in_, **kw)
        self.val[sem] += 16
        if not self.dry:
            ins.then_inc(self.sems[sem], 16)
        self._done(('d', sem, self.val[sem]), reads, writes)


def build(ntiles=NTILES):
    dry = _build(ntiles, None)
    return _build(ntiles, dry)


def _build(ntiles, needed):
    nc = bass.Bass("TRN2", target_bir_lowering=False)
    xT_d = nc.dram_tensor("xT", [D, XW], F32, kind="ExternalInput").ap()
    invc_d = nc.dram_tensor("invc", [128, 4 * NTOK], F32, kind="ExternalInput").ap()
    vecs_d = nc.dram_tensor("vecs", [128, 44], F32, kind="ExternalInput").ap()
    cst_d = nc.dram_tensor("cst", [128, 256], F32, kind="ExternalInput").ap()
    gf_d = nc.dram_tensor("gfrep", [128, D], F32, kind="ExternalInput").ap()
    win_d = nc.dram_tensor("win", [16, 128, 1024], F32, kind="ExternalInput").ap()
    wout_d = nc.dram_tensor("wout", [8, 128, 1024], F32, kind="ExternalInput").ap()
    wq_d = nc.dram_tensor("wq", [16, 128, 1024], F32, kind="ExternalInput").ap()
    keys_d = nc.dram_tensor("keys", [2, 128, 1024], F32, kind="ExternalInput").ap()
    poolw_d = nc.dram_tensor("poolw", [128, 512], F32, kind="ExternalInput").ap()
    u_d = nc.dram_tensor("u", [128, 128, 1024], F32, kind="ExternalInput").ap()
    v_d = nc.dram_tensor("v", [128, 128, 1024], F32, kind="ExternalInput").ap()
    out_d = nc.dram_tensor("out", [NTOK, D], F32, kind="ExternalOutput").ap()
    uvbf_d = nc.dram_tensor("uvbf", [128, 128, 2048], BF16, kind="Internal").ap()
    fwbf_d = nc.dram_tensor("fwbf", [40, 128, 1024], BF16, kind="Internal").ap()

    S = Sync(nc, needed)
    A = nc.alloc_sbuf_tensor
    hT = A("hT", [128, 8, TW], F32)
    rstd = A("rstd", [128, TW], F32)
    big = A("big", [128, 16 * TW], F32)
    zt = A("zt", [128, TW], F32)
    t1 = A("t1", [128, TW], F32)
    sA = A("sA", [128, TW], F32)
    sB = A("sB", [128, TW], F32)
    mixed = A("mixed", [128, TS], BF16)
    yT = A("yT", [128, 8, TS], BF16)
    h1T = [A("h1T%d" % i, [128, 8, TS], F32) for i in range(2)]
    xn2 = [A("xn2_%d" % i, [128, 8, TS], BF16) for i in range(2)]
    qT = A("qT", [128, 16, TS], BF16)
    wr = [A("wr%d" % i, [128, 2048], BF16) for i in range(NR)]
    fw = [A("fw%d" % i, [128, 1024], BF16) for i in range(3)]
    keysT = A("keysT", [128, 2048], BF16)
    poolw = A("poolw_sb", [128, 512], BF16)
    vecs = A("vecs_sb", [128, 44], F32)
    cst = A("cst_sb", [128, 256], F32)
    gfrep = A("gfrep_sb", [128, D], F32)
    ones = A("ones", [128, 128], F32)
    iota_bf = A("iota_bf", [128, 128], BF16)
    invc = A("invc_sb", [128, 4, TS], F32)
    sv = A("sv", [128, 16, 16], F32)
    si = A("si", [128, 16, 16], U32)
    sif = A("sif", [128, 16, 16], F32)
    cand = A("cand", [128, 8, 256], F32)
    tops = A("tops", [128, 8, 16], F32)
    pos = A("pos", [128, 8, 16], U32)
    au = A("au", [128, 8, 16], U32)
    bu = A("bu", [128, 8, 16], U32)
    af = A("af", [128, 8, 16], F32)
    bf = A("bf", [128, 8, 16], F32)
    ef = A("ef", [128, 8, 16], F32)
    Zt = A("Zt", [128, 8], F32)
    gate = A("gate", [128, 8, 16], F32)
    i0f = A("i0f", [128, 8, 16], F32)
    i1f = A("i1f", [128, 8, 16], F32)
    i0T = A("i0T", [128, TS], F32)
    i1T = A("i1T", [128, TS], F32)
    gT = A("gT", [128, TS], F32)
    GT = 8
    P1oh = [A("P1oh%d" % i, [128, GT, 128], BF16) for i in range(2)]
    Q0oh = [A("Q0oh%d" % i, [128, GT, 128], BF16) for i in range(2)]
    WG = A("WG", [128, 128, TS], BF16)
    Hb = [A("Hb%d" % i, [128, TS], BF16) for i in range(3)]
    Gb = [A("Gb%d" % i, [128, TS], BF16) for i in range(4)]
    ssq = A("ssq", [128, 2], F32)
    ps = [nc.alloc_psum_tensor("ps%d" % i, [128, 512], F32) for i in range(8)]

    ident = cst[:, 0:128]
    ot = big[:, 0:1024]
    h1tok = big[:, 1024:2048]
    hn = qT[:].rearrange("p c t -> p (c t)")[:, 0:8 * TW].rearrange("p (c t) -> p c t", c=8)
    iota = cst[:, 128:256]
    pT = big[:, 0:16 * TW].rearrange("p (c t) -> p c t", c=16)
    sqv = big[:, 0:8 * TW].rearrange("p (c t) -> p c t", c=8)
    Ssb = big[:, 0:2048].rearrange("p (a b) -> p a b", a=16)
    S2 = big[:, 2048:4096].rearrange("p (a b) -> p a b", a=16)
    cand2 = big[:, 0:2048].rearrange("p (a b) -> p a b", a=8)
    eq = big[:, 2048:4096].rearrange("p (h k a) -> p h k a", h=8, k=16)

    def pstride(t):
        return list(t[:].ap[0])

    state = {'fb': 0, 'wb': 0, 'wk': 0, 'fk': 0}
    FB = (6, 7)
    WB = (0, 1)

    def fbank():
        b = FB[state['fb'] % 2]
        state['fb'] += 1
        return b

    def wbank():
        b = WB[state['wb'] % 2]
        state['wb'] += 1
        return b

    pref = {}

    def fsrc(idx):
        if idx < 16:
            return win_d[idx]
        if idx < 24:
            return wout_d[idx - 16]
        return wq_d[idx - 24]

    def fissue(ti, idx):
        k = state['fk'] % 3
        state['fk'] += 1
        cres = 'fwbf%d' % idx
        if ti == 0:
            S.dma('pool', fw[k][:], fsrc(idx), writes=['fw%d' % k], sem='d_fw%d' % k)
            S.dma('sp', fwbf_d[idx], fw[k][:], reads=['fw%d' % k], writes=[cres], sem='d_fsb%d' % k)
        else:
            S.dma('pool', fw[k][:], fwbf_d[idx], reads=[cres], writes=['fw%d' % k], sem='d_fw%d' % k)
        pref[(ti, idx)] = k

    def fload(src, ti, idx):
        if (ti, idx) not in pref:
            fissue(ti, idx)
        k = pref.pop((ti, idx))
        for nxt in (idx + 1, idx + 2):
            if nxt < 40 and (ti, nxt) not in pref:
                fissue(ti, nxt)
        return k

    S.dma('sp', vecs[:], vecs_d, writes=['vecs'], sem='d_vecs')
    S.dma('sp', cst[:], cst_d, writes=['cst'], sem='d_cst')
    S.dma('sp', gfrep[:], gf_d, writes=['gfrep'], sem='d_gf')
    S.dma('pool', keysT[:, 0:1024], keys_d[0], writes=['keysT'], sem='d_k')
    S.dma('pool', keysT[:, 1024:2048], keys_d[1], writes=['keysT'], sem='d_k')
    S.dma('pool', poolw[:], poolw_d, writes=['poolw'], sem='d_pw')
    S.op('dve', lambda e: e.memset(ones[:], 1.0), writes=['ones'])
    S.op('dve', lambda e: e.tensor_copy(iota_bf[:], cst[:, 128:256]), reads=['cst'], writes=['iota_bf'])

    xT_v = xT_d.rearrange("(c p) t -> p c t", p=128)
    invc_v = invc_d.rearrange("p (g t) -> p g t", g=4)

    def rmsnorm_units(units, src, srcres, W, gcol, dst, dstres):
        def u_sq():
            S.op('act', lambda e: e.activation(sqv[:, :, 0:W], src[:], AF.Square), reads=[srcres], writes=['big'])
        def u_ssq():
            b = fbank()
            for dc in range(8):
                S.op('pe', lambda e: e.matmul(ps[b][:, 0:W], ones[:], sqv[:, dc, 0:W], start=(dc == 0), stop=(dc == 7)),
                     reads=['ones', 'big'], writes=['ps%d' % b])
            S.op('act', lambda e: e.activation(rstd[:, 0:W], ps[b][:, 0:W], AF.Sqrt, bias=EPS, scale=1.0 / D),
                 reads=['ps%d' % b], writes=['rstd'])
            S.op('dve', lambda e: e.reciprocal(rstd[:, 0:W], rstd[:, 0:W]), reads=['rstd'], writes=['rstd'])
        def u_scale(dc0):
            for dc in range(dc0, dc0 + 4):
                S.op('dve', lambda e: e.scalar_tensor_tensor(dst[:, dc, :], src[:, dc, :], vecs[:, gcol + dc:gcol + dc + 1],
                                                            rstd[:, 0:W], ALU.mult, ALU.mult),
                     reads=[srcres, 'rstd', 'vecs'], writes=[dstres])
        units.append(u_sq)
        units.append(u_ssq)
        units.append(lambda: u_scale(0))
        units.append(lambda: u_scale(4))

    def front_units(ti):
        par = ti % 2
        t0 = ti * TS
        h1 = h1T[par]
        xn = xn2[par]
        h1res = 'h1T%d' % par
        xnres = 'xn2_%d' % par
        units = []

        def u_load():
            S.dma('sp', hT[:], xT_v[:, :, t0:t0 + TW], writes=['hT'], sem='d_x')
            S.dma('sp', invc[:], invc_v[:, :, t0:t0 + TS], writes=['invc'], sem='d_i')
        units.append(u_load)
        rmsnorm_units(units, hT, 'hT', TW, 0, hn, 'qT')

        def u_inproj(cc):
            k = fload(win_d[cc], ti, cc)
            b = fbank()
            for dc in range(8):
                S.op('pe', lambda e: e.matmul(ps[b][:, 0:TW], fw[k][:, dc * 128:(dc + 1) * 128], hn[:, dc, :],
                                              start=(dc == 0), stop=(dc == 7)),
                     reads=['fw%d' % k, 'qT'], writes=['ps%d' % b])
            S.op('act', lambda e: e.copy(pT[:, cc, :], ps[b][:, 0:TW]), reads=['ps%d' % b], writes=['big'])
        for cc in range(16):
            units.append(lambda cc=cc: u_inproj(cc))

        def u_conv(c):
            S.op('dve', lambda e: e.tensor_tensor(zt[:], pT[:, 8 + c, :], pT[:, c, :], ALU.mult), reads=['big'], writes=['zt'])
            S.op('dve', lambda e: e.tensor_scalar(t1[:, 0:TS], zt[:, 7:7 + TS], vecs[:, 24 + c:25 + c], None, ALU.mult),
                 reads=['zt', 'vecs'], writes=['t1'])
            S.op('dve', lambda e: e.scalar_tensor_tensor(t1[:, 0:TS], zt[:, 8:8 + TS], vecs[:, 28 + c:29 + c], t1[:, 0:TS], ALU.mult, ALU.add),
                 reads=['zt', 'vecs', 't1'], writes=['t1'])
            S.op('dve', lambda e: e.scalar_tensor_tensor(t1[:, 0:TS], zt[:, 9:9 + TS], vecs[:, 32 + c:33 + c], t1[:, 0:TS], ALU.mult, ALU.add),
                 reads=['zt', 'vecs', 't1'], writes=['t1'])
            S.op('dve', lambda e: e.scalar_tensor_tensor(yT[:, c, :], t1[:, 0:TS], vecs[:, 36 + c:37 + c], pT[:, 4 + c, 8:8 + TS], ALU.add, ALU.mult),
                 reads=['t1', 'vecs', 'big'], writes=['yT'])
        for c in range(4):
            units.append(lambda c=c: u_conv(c))

        def u_pool(g, w):
            x = pT[:, 12 + g, :]
            S.op('dve', lambda e: e.tensor_tensor(sA[:, 1:272], x[:, 0:271], x[:, 1:272], ALU.add), reads=['big'], writes=['sA'])
            fin = sA
            if w >= 4:
                S.op('dve', lambda e: e.tensor_tensor(sB[:, 2:271], sA[:, 1:270], sA[:, 3:272], ALU.add), reads=['sA'], writes=['sB'])
                fin = sB
            if w >= 8:
                S.op('dve', lambda e: e.tensor_tensor(sA[:, 4:269], sB[:, 2:267], sB[:, 6:271], ALU.add), reads=['sB'], writes=['sA'])
                fin = sA
            if w >= 16:
                S.op('dve', lambda e: e.tensor_tensor(sB[:, 8:265], sA[:, 4:261], sA[:, 12:269], ALU.add), reads=['sA'], writes=['sB'])
                fin = sB
            S.op('dve', lambda e: e.tensor_tensor(zt[:, 0:TS], fin[:, 8:8 + TS], invc[:, g, :], ALU.mult),
                 reads=['sA', 'sB', 'invc'], writes=['zt'])
            S.op('dve', lambda e: e.tensor_tensor(mixed[:], zt[:, 0:TS], x[:, 8:8 + TS], ALU.subtract),
                 reads=['zt', 'big'], writes=['mixed'])
            b = fbank()
            S.op('pe', lambda e: e.matmul(ps[b][:, 0:TS], poolw[:, g * 128:(g + 1) * 128], mixed[:], start=True, stop=True),
                 reads=['poolw', 'mixed'], writes=['ps%d' % b])
            S.op('dve', lambda e: e.tensor_scalar(yT[:, 4 + g, :], ps[b][:, 0:TS], vecs[:, 40 + g:41 + g], None, ALU.mult),
                 reads=['ps%d' % b, 'vecs'], writes=['yT'])
        for g, w in enumerate((2, 4, 8, 16)):
            units.append(lambda g=g, w=w: u_pool(g, w))

        def u_outproj(dd):
            k = fload(wout_d[dd], ti, 16 + dd)
            b = fbank()
            for cc in range(8):
                S.op('pe', lambda e: e.matmul(ps[b][:, 0:TS], fw[k][:, cc * 128:(cc + 1) * 128], yT[:, cc, :],
                                              start=(cc == 0), stop=(cc == 7)),
                     reads=['fw%d' % k, 'yT'], writes=['ps%d' % b])
            S.op('dve', lambda e: e.tensor_tensor(h1[:, dd, :], hT[:, dd, 8:8 + TS], ps[b][:, 0:TS], ALU.add),
                 reads=['hT', 'ps%d' % b], writes=[h1res])
        for dd in range(8):
            units.append(lambda dd=dd: u_outproj(dd))
        rmsnorm_units(units, h1, h1res, TS, 8, xn, xnres)

        def u_q(cc):
            k = fload(wq_d[cc], ti, 24 + cc)
            b = fbank()
            for dc in range(8):
                S.op('pe', lambda e: e.matmul(ps[b][:, 0:TS], fw[k][:, dc * 128:(dc + 1) * 128], xn[:, dc, :],
                                              start=(dc == 0), stop=(dc == 7)),
                     reads=['fw%d' % k, xnres], writes=['ps%d' % b])
            S.op('act', lambda e: e.copy(qT[:, cc, :], ps[b][:, 0:TS]), reads=['ps%d' % b], writes=['qT'])
        for cc in range(16):
            units.append(lambda cc=cc: u_q(cc))

        ts_units = [[], []]
        tr_units = [None, None]
        for ts in range(2):
            tsl = slice(ts * 128, (ts + 1) * 128)
            units_ts = ts_units[ts]

            def u_scores(kq, tsl=tsl):
                b = fbank()
                for c4 in range(4):
                    cc = kq * 4 + c4
                    S.op('pe', lambda e: e.matmul(ps[b][:, c4 * 128:(c4 + 1) * 128], qT[:, cc, tsl],
                                                  keysT[:, cc * 128:(cc + 1) * 128], start=True, stop=True),
                         reads=['qT', 'keysT'], writes=['ps%d' % b])
                S.op('act', lambda e: e.copy(big[:, kq * 512:(kq + 1) * 512], ps[b][:]), reads=['ps%d' % b], writes=['big'])
            for kq in range(4):
                units_ts.append(lambda kq=kq, f=u_scores: f(kq))

            def u_topk(cc):
                S.op('dve', lambda e: e.max(sv[:, cc, 0:8], Ssb[:, cc, :]), reads=['big'], writes=['sv'])
                S.op('dve', lambda e: e.max_index(si[:, cc, 0:8], sv[:, cc, 0:8], Ssb[:, cc, :]), reads=['big', 'sv'], writes=['si'])
                S.op('dve', lambda e: e.match_replace(S2[:, cc, :], sv[:, cc, 0:8], Ssb[:, cc, :], -1e30), reads=['sv'], writes=['big'])
                S.op('dve', lambda e: e.max(sv[:, cc, 8:16], S2[:, cc, :]), reads=['big'], writes=['sv'])
                S.op('dve', lambda e: e.max_index(si[:, cc, 8:16], sv[:, cc, 8:16], S2[:, cc, :]), reads=['big', 'sv'], writes=['si'])
            for cc in range(16):
                units_ts.append(lambda cc=cc: u_topk(cc))

            def u_cand(hh):
                if hh == 0:
                    S.op('dve', lambda e: e.tensor_copy(sif[:], si[:]), reads=['si'], writes=['sif'])
                pp = pstride(sv)
                c_in0 = AP(tensor=sv, offset=hh * 128, ap=[pp, [32, 4], [1, 16], [0, 16]])
                c_in1 = AP(tensor=sv, offset=hh * 128 + 16, ap=[pp, [32, 4], [0, 16], [1, 16]])
                S.op('dve', lambda e: e.tensor_tensor(cand[:, hh * 4:hh * 4 + 4, :].rearrange("p h (a b) -> p h a b", a=16), c_in0, c_in1, ALU.add),
                     reads=['sv'], writes=['cand'])
            units_ts.append(lambda: u_cand(0))
            units_ts.append(lambda: u_cand(1))

            def u_top2(h):
                S.op('dve', lambda e: e.max(tops[:, h, 0:8], cand[:, h, :]), reads=['cand'], writes=['tops'])
                S.op('dve', lambda e: e.max_index(pos[:, h, 0:8], tops[:, h, 0:8], cand[:, h, :]), reads=['cand', 'tops'], writes=['pos'])
                S.op('dve', lambda e: e.match_replace(cand2[:, h, :], tops[:, h, 0:8], cand[:, h, :], -1e30), reads=['cand', 'tops'], writes=['big'])
                S.op('dve', lambda e: e.max(tops[:, h, 8:16], cand2[:, h, :]), reads=['big'], writes=['tops'])
                S.op('dve', lambda e: e.max_index(pos[:, h, 8:16], tops[:, h, 8:16], cand2[:, h, :]), reads=['big', 'tops'], writes=['pos'])
            for h in range(8):
                units_ts.append(lambda h=h: u_top2(h))

            def u_gates(part):
                if part == 0:
                    mx = AP(tensor=tops, offset=0, ap=[pstride(tops), [16, 8], [0, 16]])
                    S.op('dve', lambda e: e.tensor_tensor(ef[:], tops[:], mx, ALU.subtract), reads=['tops'], writes=['ef'])
                    S.op('act', lambda e: e.activation(ef[:], ef[:], AF.Exp), reads=['ef'], writes=['ef'])
                    S.op('dve', lambda e: e.tensor_single_scalar(au[:], pos[:], 4, op=ALU.logical_shift_right), reads=['pos'], writes=['au'])
                    S.op('dve', lambda e: e.tensor_single_scalar(bu[:], pos[:], 15, op=ALU.bitwise_and), reads=['pos'], writes=['bu'])
                    S.op('dve', lambda e: e.tensor_copy(af[:], au[:]), reads=['au'], writes=['af'])
                    S.op('dve', lambda e: e.tensor_copy(bf[:], bu[:]), reads=['bu'], writes=['bf'])
                else:
                    S.op('dve', lambda e: e.tensor_reduce(Zt[:], ef[:], AX.X, ALU.add), reads=['ef'], writes=['Zt'])
                    S.op('dve', lambda e: e.reciprocal(Zt[:], Zt[:]), reads=['Zt'], writes=['Zt'])
                    S.op('dve', lambda e: e.tensor_tensor(gate[:], ef[:], Zt[:].unsqueeze(2).to_broadcast([128, 8, 16]), ALU.mult),
                         reads=['ef', 'Zt'], writes=['gate'])
            units_ts.append(lambda: u_gates(0))
            units_ts.append(lambda: u_gates(1))

            def u_idx(xf, off, dst, hh, stage):
                hs = slice(hh * 4, hh * 4 + 4)
                eqh = eq[:, hs]
                if stage == 0:
                    io16 = AP(tensor=cst, offset=128, ap=[pstride(cst), [0, 4], [0, 16], [1, 16]])
                    xb = AP(tensor=xf, offset=hh * 64, ap=[pstride(xf), [16, 4], [1, 16], [0, 16]])
                    S.op('dve', lambda e: e.tensor_tensor(eqh, io16, xb, ALU.is_equal), reads=['cst', 'af', 'bf'], writes=['big'])
                elif stage == 1:
                    sb = AP(tensor=sif, offset=off + hh * 128, ap=[pstride(sif), [32, 4], [0, 16], [1, 16]])
                    S.op('dve', lambda e: e.tensor_tensor(eqh, eqh, sb, ALU.mult), reads=['sif'], writes=['big'])
                else:
                    S.op('dve', lambda e: e.tensor_reduce(dst[:, hs, :], eqh, AX.X, ALU.add), reads=['big'], writes=[dst.name])
            for (xf_, off_, dst_) in ((af, 0, i0f), (bf, 16, i1f)):
                for hh_ in range(2):
                    for st_ in range(3):
                        units_ts.append(lambda a=xf_, b=off_, c=dst_, d=hh_, f=st_: u_idx(a, b, c, d, f))

            def u_tr(tsl=tsl):
                b = fbank()
                for n, src in enumerate((i0f, i1f, gate)):
                    S.op('pe', lambda e: e.transpose(ps[b][:, n * 128:(n + 1) * 128], src[:].rearrange("p h k -> p (h k)"), ident),
                         reads=[src.name, 'cst'], writes=['ps%d' % b])
                for n, dst in enumerate((i0T, i1T, gT)):
                    S.op('act', lambda e: e.copy(dst[:, tsl], ps[b][:, n * 128:(n + 1) * 128]),
                         reads=['ps%d' % b], writes=[dst.name])
            tr_units[ts] = u_tr
        ts0, ts1 = ts_units
        merged = units + ts0 + ts1[:8] + [tr_units[0]] + ts1[8:] + [tr_units[1]]
        return merged[:21], merged[21:]

    def wbuild(ti, side=()):
        side = list(side)
        for gq in range(TS // GT):
            tq = gq * GT
            bi = gq % 2
            P1, Q0 = P1oh[bi], Q0oh[bi]
            tag = '%d_%d' % (ti, gq)
            for t in range(GT):
                tok = tq + t
                wx = ['ohg%d' % bi] if t == 0 else []
                S.op('dve', lambda e: e.tensor_scalar(P1[:, t, :], iota_bf[:], i1T[:, tok:tok + 1], None, ALU.is_equal),
                     reads=['iota_bf', 'i1T'], writes=wx)
                S.op('dve', lambda e: e.tensor_scalar(Q0[:, t, :], iota_bf[:], i0T[:, tok:tok + 1], gT[:, tok:tok + 1], ALU.is_equal, ALU.mult),
                     reads=['iota_bf', 'i0T', 'gT'], writes=['oh_%s_%d' % (tag, t)])
            for q4 in range(GT // 4):
                b = wbank()
                quad = ['oh_%s_%d' % (tag, q4 * 4 + tt) for tt in range(4)]
                for tt in range(4):
                    t = q4 * 4 + tt
                    S.op('pe', lambda e: e.matmul(ps[b][:].rearrange("p (i t) -> p t i", t=4)[:, tt, :], Q0[:, t, :], P1[:, t, :], start=True, stop=True),
                         reads=quad + ['ohg%d' % bi], writes=['ps%d' % b])
                tg = tq + q4 * 4
                S.op('act', lambda e: e.copy(WG[:, :, tg:tg + 4], ps[b][:].rearrange("p (i t) -> p i t", t=4)),
                     reads=['ps%d' % b], writes=['WGw'])
            if side:
                side.pop(0)()
        while side:
            side.pop(0)()

    def dense(ti):
        par = ti % 2
        xn = xn2[par]
        xnres = 'xn2_%d' % par
        SKEW = 2
        slots = {}

        def load(i1):
            k = state['wk'] % NR
            state['wk'] += 1
            cres = 'uvbf%d' % i1
            if ti == 0:
                S.dma('pool', wr[k][:, 0:1024], u_d[i1], writes=['wr%d' % k], sem='d_wr%d' % k)
                S.dma('pool', wr[k][:, 1024:2048], v_d[i1], writes=['wr%d' % k], sem='d_wr%d' % k)
                S.dma('sp', uvbf_d[i1], wr[k][:], reads=['wr%d' % k], writes=[cres], sem='d_sb%d' % k)
            else:
                S.dma('sp', wr[k][:], uvbf_d[i1], reads=[cres], writes=['wr%d' % k], sem='d_wrh%d' % k)
            slots[i1] = k
            return k

        for step in range(128 + SKEW):
            i1 = step
            if i1 < 128:
                k = load(i1)
                b = i1 % 2
                for dc in range(8):
                    S.op('pe', lambda e: e.matmul(ps[b][:, 0:TS], wr[k][:, dc * 128:(dc + 1) * 128], xn[:, dc, :],
                                                  start=(dc == 0), stop=(dc == 7)),
                         reads=['wr%d' % k, xnres], writes=['ps%d' % b])
                hb = Hb[i1 % 3]
                gbuf = Gb[i1 % 4]
                S.op('act', lambda e: e.activation(hb[:], ps[b][:, 0:TS], AF.Gelu), reads=['ps%d' % b], writes=['Hb%d' % (i1 % 3)])
                S.op('dve', lambda e: e.tensor_tensor(gbuf[:], hb[:], WG[:, i1, :], ALU.mult),
                     reads=['Hb%d' % (i1 % 3), 'WGw'], writes=['Gb%d' % (i1 % 4)])
            j1 = step - SKEW
            if j1 >= 0:
                k = slots.pop(j1)
                gbuf = Gb[j1 % 4]
                for ts in range(2):
                    for dh in range(2):
                        bk = 2 + ts * 2 + dh
                        S.op('pe', lambda e: e.matmul(ps[bk][:], gbuf[:, ts * 128:(ts + 1) * 128], wr[k][:, 1024 + dh * 512:1024 + (dh + 1) * 512],
                                                      start=(j1 == 0), stop=(j1 == 127)),
                             reads=['wr%d' % k, 'Gb%d' % (j1 % 4)], writes=['ps%d' % bk])
            yield

    def final_units(ti):
        par = ti % 2
        h1 = h1T[par]
        h1res = 'h1T%d' % par
        t0 = ti * TS
        units = []

        def u_tp(ts, hf):
            b = fbank()
            for c4 in range(4):
                dc = hf * 4 + c4
                S.op('pe', lambda e: e.transpose(ps[b][:, c4 * 128:(c4 + 1) * 128], h1[:, dc, ts * 128:(ts + 1) * 128], ident),
                     reads=[h1res, 'cst'], writes=['ps%d' % b])
            S.op('act', lambda e: e.copy(h1tok[:, hf * 512:(hf + 1) * 512], ps[b][:]), reads=['ps%d' % b], writes=['big'])

        def u_add(ts):
            for dh in range(2):
                bk = 2 + ts * 2 + dh
                S.op('dve', lambda e: e.tensor_tensor(h1tok[:, dh * 512:(dh + 1) * 512], h1tok[:, dh * 512:(dh + 1) * 512], ps[bk][:], ALU.add),
                     reads=['ps%d' % bk], writes=['big'])

        def u_norm(ts):
            S.op('act', lambda e: e.activation(ot, h1tok, AF.Square, accum_out=ssq[:, 0:1]), reads=[], writes=['big', 'ssq'])
            S.op('act', lambda e: e.activation(ssq[:, 1:2], ssq[:, 0:1], AF.Sqrt, bias=EPS, scale=1.0 / D), reads=['ssq'], writes=['ssq'])
            S.op('dve', lambda e: e.reciprocal(ssq[:, 1:2], ssq[:, 1:2]), reads=['ssq'], writes=['ssq'])
            S.op('dve', lambda e: e.scalar_tensor_tensor(ot, h1tok, ssq[:, 1:2], gfrep[:], ALU.mult, ALU.mult),
                 reads=['ssq', 'gfrep'], writes=['big'])
            r0 = t0 + ts * 128
            S.dma('sp', out_d[r0:r0 + 128, :], ot, reads=['big'], sem='d_out')

        for ts in range(2):
            units.append(lambda ts=ts: u_tp(ts, 0))
            units.append(lambda ts=ts: u_tp(ts, 1))
            units.append(lambda ts=ts: u_add(ts))
            units.append(lambda ts=ts: u_norm(ts))
        return units

    fA, fB = front_units(0)
    for u in fA + fB:
        u()
    nA, nB = front_units(1) if ntiles > 1 else ([], [])
    wbuild(0, nA)
    for ti in range(ntiles):
        side = nB
        nside = len(side)
        done = 0
        nsteps = 131
        cost = 0.0
        per_step = state.get('side_cost', 650.0) / float(nsteps - 14)
        for step, _ in enumerate(dense(ti)):
            while done < nside and cost < (step + 1) * per_step:
                a_d, a_p = S.idx['dve'], S.idx['pe']
                side[done]()
                done += 1
                cost += (S.idx['dve'] - a_d) + 0.2 * (S.idx['pe'] - a_p)
        while done < nside:
            a_d, a_p = S.idx['dve'], S.idx['pe']
            side[done]()
            done += 1
            cost += (S.idx['dve'] - a_d) + 0.2 * (S.idx['pe'] - a_p)
        if nside:
            state['side_cost'] = cost
        if ti + 1 < ntiles:
            nA, nB = front_units(ti + 2) if ti + 2 < ntiles else ([], [])
            wbuild(ti + 1, final_units(ti) + nA)
        else:
            for u in final_units(ti):
                u()

    if S.dry:
        return S.needed
    nc.sync.wait_ge(S.sems['d_out'], S.val['d_out'])
    return nc


def _prep_weights(inputs):
    f = lambda a: np.ascontiguousarray(np.asarray(a, dtype=np.float32))
    w_in = f(inputs["w_in"])
    win = w_in.reshape(8, 128, 16, 128).transpose(2, 1, 0, 3).reshape(16, 128, 1024)
    w_out = f(inputs["w_out"])
    wout = w_out.reshape(8, 128, 8, 128).transpose(2, 1, 0, 3).reshape(8, 128, 1024)
    w_q = f(inputs["peer_w_q"])
    wq = w_q.reshape(8, 128, 16, 128).transpose(2, 1, 0, 3).reshape(16, 128, 1024)
    keys = f(inputs["peer_keys"]).reshape(16, 128, 128)
    keys_l = keys.transpose(2, 0, 1).reshape(128, 2, 1024).transpose(1, 0, 2)
    poolw = f(inputs["pool_w"]).transpose(1, 0, 2).reshape(128, 512)
    u = f(inputs["peer_u"]).reshape(128, 128, 8, 128)
    u_l = u.transpose(1, 3, 2, 0).reshape(128, 128, 1024)
    v = f(inputs["peer_v"]).reshape(128, 128, 1024)
    v_l = v.transpose(1, 0, 2)
    vecs = np.zeros((128, 44), np.float32)
    vecs[:, 0:8] = f(inputs["norm1_g"]).reshape(8, 128).T
    vecs[:, 8:16] = f(inputs["norm2_g"]).reshape(8, 128).T
    vecs[:, 16:24] = f(inputs["final_norm_g"]).reshape(8, 128).T
    cw = f(inputs["conv_w"])
    for kk in range(3):
        vecs[:, 24 + 4 * kk:28 + 4 * kk] = cw[kk].reshape(4, 128).T
    vecs[:, 36:40] = f(inputs["conv_b"]).reshape(4, 128).T
    vecs[:, 40:44] = f(inputs["pool_scale"]).reshape(4, 128).T
    cst = np.zeros((128, 256), np.float32)
    cst[:, 0:128] = np.eye(128, dtype=np.float32)
    cst[:, 128:256] = np.arange(128, dtype=np.float32)[None, :]
    c = np.ascontiguousarray
    gfrep = np.ascontiguousarray(np.broadcast_to(f(inputs["final_norm_g"]).reshape(1, D), (128, D)))
    return dict(win=c(win), wout=c(wout), wq=c(wq), keys=c(keys_l), poolw=c(poolw), u=c(u_l), v=c(v_l),
                vecs=vecs, cst=cst, gfrep=gfrep)


def _prep_core(inputs, b, half):
    x = np.asarray(inputs["x"], dtype=np.float32)
    meta = np.asarray(inputs["meta_tokens"], dtype=np.float32)
    L = 16 + x.shape[1]
    p0 = 16 + NTOK * half
    lo, hi = p0 - HALO, p0 + NTOK + HALO
    rows = np.zeros((XW, D), np.float32)
    full_lo = lo
    for (a, bnd) in ((lo, min(hi, 16)),):
        if a < bnd:
            rows[a - full_lo:bnd - full_lo] = meta[a:bnd]
    a = max(lo, 16)
    bnd = min(hi, L)
    rows[a - full_lo:bnd - full_lo] = x[b, a - 16:bnd - 16]
    xT = np.ascontiguousarray(rows.T)
    p = p0 + np.arange(NTOK)
    inv = np.zeros((4, NTOK), np.float32)
    for g, w in enumerate((2, 4, 8, 16)):
        cnt = np.minimum(p + w // 2, L) - np.maximum(p - w // 2, 0)
        inv[g] = 1.0 / cnt.astype(np.float32)
    invc = np.ascontiguousarray(np.broadcast_to(inv.reshape(1, 4 * NTOK), (128, 4 * NTOK)))
    return dict(xT=xT, invc=invc)


def kernel(**inputs):
    wts = _prep_weights(inputs)
    in_maps = []
    for core in range(8):
        b, half = core // 2, core % 2
        m = dict(wts)
        m.update(_prep_core(inputs, b, half))
        in_maps.append(m)
    nc = build()
    res = run_bass_kernel_spmd(nc, in_maps, core_ids=list(range(8)))
    B = 4
    out = np.empty((B, 2 * NTOK, D), np.float32)
    for core in range(8):
        b, half = core // 2, core % 2
        out[b, half * NTOK:(half + 1) * NTOK, :] = res.results[core]["out"]
    return out
```
